# Optimizing a Trainium2 kernel written in Bass

```python
import math
import jax, jax.numpy as jnp
from jax import lax
import numpy as np

D_MODEL = 1024
BATCH = 2
SEQ = 8192
DEPTH = 2

QBLK = 128
NORM_EPS = 1e-6
ROPE_BASE = 10000.0
MAX_POS_OFFSET = 4096

A_HEADS = 8
A_HEAD_DIM = 64
IDX_HEADS = 4
IDX_DIM = 64
DSA_TOPK = 256
A_WIDTH = A_HEADS * A_HEAD_DIM
B_HEADS = 4
B_QK_DIM = 64
B_V_DIM = 128
RET_CHUNK = 128
B_WIDTH = B_HEADS * B_V_DIM
C_HEADS = 4
C_QK_DIM = 64
C_V_DIM = 128
C_WIDTH = C_HEADS * C_V_DIM
D_HEADS = 8
D_NOPE_DIM = 64
D_ROPE_DIM = 32
D_V_DIM = 64
D_Q_LORA = 256
D_KV_LORA = 128
D_WIDTH = D_HEADS * D_V_DIM

EVEN_MIX_WIDTH = A_WIDTH + B_WIDTH
ODD_MIX_WIDTH = C_WIDTH + D_WIDTH
EVEN_WIDTHS = (A_WIDTH, A_WIDTH, A_WIDTH, A_WIDTH,
               IDX_HEADS * IDX_DIM, IDX_DIM, IDX_HEADS,
               B_HEADS * B_QK_DIM, B_HEADS * B_QK_DIM,
               B_WIDTH, B_WIDTH)
ODD_WIDTHS = (C_HEADS * 2 * C_QK_DIM, C_HEADS * 2 * C_QK_DIM,
              C_WIDTH, C_WIDTH,
              D_Q_LORA, D_KV_LORA, D_ROPE_DIM, D_WIDTH)
EVEN_IN = sum(EVEN_WIDTHS)
ODD_IN = sum(ODD_WIDTHS)

kernel_name = 'hybrid_dsa_retention_diffattn_mla'


def split_columns(h, widths):
    out, start = [], 0
    for w in widths:
        out.append(h[..., start:start + w])
        start += w
    return out


def rms_norm(x, gain=None):
    xf = x.astype(jnp.float32)
    y = xf * lax.rsqrt(jnp.mean(xf * xf, axis=-1, keepdims=True) + NORM_EPS)
    if gain is not None:
        y = y * gain.astype(jnp.float32)
    return y.astype(x.dtype)


def head_group_norm(x):
    xf = x.astype(jnp.float32)
    mu = jnp.mean(xf, axis=-1, keepdims=True)
    var = jnp.mean(jnp.square(xf - mu), axis=-1, keepdims=True)
    return ((xf - mu) * lax.rsqrt(var + NORM_EPS)).astype(x.dtype)


def rotary(x, positions):
    half = x.shape[-1] // 2
    inv_freq = ROPE_BASE ** (-jnp.arange(half, dtype=jnp.float32) / half)
    ang = positions.astype(jnp.float32)[:, :, None, None] * inv_freq
    cos, sin = jnp.cos(ang), jnp.sin(ang)
    xf = x.astype(jnp.float32)
    x1, x2 = xf[..., :half], xf[..., half:]
    return jnp.concatenate([x1 * cos - x2 * sin, x1 * sin + x2 * cos], axis=-1).astype(x.dtype)


def to_blocks(a, nb):
    b = a.shape[0]
    return a.reshape((b, nb, QBLK) + a.shape[2:]).swapaxes(0, 1)


def from_blocks(a):
    nb, b, q = a.shape[:3]
    return a.swapaxes(0, 1).reshape((b, nb * q) + a.shape[3:])


def causal_attention(q, k, v, scale):
    t = q.shape[1]
    nb = t // QBLK
    kpos = jnp.arange(t)

    def block(args):
        qb, i = args
        qpos = i * QBLK + jnp.arange(QBLK)
        s = jnp.einsum('bqhd,bkhd->bhqk', qb, k).astype(jnp.float32) * scale
        s = jnp.where((kpos[None, :] <= qpos[:, None])[None, None], s, -jnp.inf)
        p = jax.nn.softmax(s, axis=-1).astype(v.dtype)
        return jnp.einsum('bhqk,bkhd->bqhd', p, v)

    out = lax.map(block, (to_blocks(q, nb), jnp.arange(nb)))
    return from_blocks(out)


def dsa_attention(q, k, v, q_idx, k_idx, w_idx):
    b, t, h, d = q.shape
    topk = min(DSA_TOPK, t // 4)
    nb = t // QBLK
    kpos = jnp.arange(t)
    w_idx = w_idx * (IDX_HEADS ** -0.5 * IDX_DIM ** -0.5)
    gather = jax.vmap(lambda a, ix: a[ix])

    def block(args):
        qb, qib, wb, i = args
        qpos = i * QBLK + jnp.arange(QBLK)
        causal = kpos[None, :] <= qpos[:, None]
        rel = jax.nn.relu(jnp.einsum('bqhd,bsd->bqhs', qib, k_idx))
        score = jnp.einsum('bqh,bqhs->bqs', wb, rel).astype(jnp.float32)
        score = jnp.where(causal[None], score, -jnp.inf)
        _, sel = lax.top_k(score, topk)
        valid = sel <= qpos[None, :, None]
        ks = gather(k, sel).reshape(b, QBLK, topk, h, d)
        vs = gather(v, sel).reshape(b, QBLK, topk, h, d)
        s = jnp.einsum('bqhd,bqkhd->bqhk', qb, ks).astype(jnp.float32) * (d ** -0.5)
        s = jnp.where(valid[:, :, None, :], s, -jnp.inf)
        p = jax.nn.softmax(s, axis=-1).astype(vs.dtype)
        return jnp.einsum('bqhk,bqkhd->bqhd', p, vs)

    out = lax.map(block, (to_blocks(q, nb), to_blocks(q_idx, nb), to_blocks(w_idx, nb), jnp.arange(nb)))
    return from_blocks(out).reshape(b, t, h * d)


def retention(q, k, v):
    b, t, h, dk = q.shape
    dv = v.shape[-1]
    c = RET_CHUNK
    n = t // c
    gammas = 1.0 - 2.0 ** (-5.0 - jnp.arange(h, dtype=jnp.float32))
    log_g = jnp.log(gammas)
    q = q.reshape(b, n, c, h, dk)
    k = k.reshape(b, n, c, h, dk)
    v = v.reshape(b, n, c, h, dv)
    idx = jnp.arange(c)
    diff = idx[:, None] - idx[None, :]
    decay = jnp.where(diff[None] >= 0, jnp.exp(diff[None].astype(jnp.float32) * log_g[:, None, None]), 0.0)
    s = jnp.einsum('bnihd,bnjhd->bnhij', q, k) * decay
    intra = jnp.einsum('bnhij,bnjhe->bnihe', s, v)
    zeta = jnp.exp((c - 1 - idx).astype(jnp.float32)[:, None] * log_g[None, :])
    xi = jnp.exp((idx + 1).astype(jnp.float32)[:, None] * log_g[None, :])
    u = jnp.einsum('bnjhd,jh,bnjhe->nbhde', k, zeta, v)
    g_chunk = jnp.exp(c * log_g)[None, :, None, None]

    def step(state, inc):
        return g_chunk * state + inc, state

    _, s_prev = lax.scan(step, jnp.zeros((b, h, dk, dv), u.dtype), u)
    cross = jnp.einsum('bnihd,ih,nbhde->bnihe', q, xi, s_prev)
    return (intra + cross).reshape(b, t, h, dv).astype(v.dtype)


def even_mixer(h, positions, w_in, w_out):
    b, t, _ = h.shape
    (aq, ak, av, ag, iq, ik, iw, bq, bk, bv, bg) = split_columns(h @ w_in, EVEN_WIDTHS)
    a = dsa_attention(aq.reshape(b, t, A_HEADS, A_HEAD_DIM), ak, av,
                      iq.reshape(b, t, IDX_HEADS, IDX_DIM), ik, iw)
    a = a * jax.nn.silu(ag)
    q = rotary(bq.reshape(b, t, B_HEADS, B_QK_DIM), positions)
    k = rotary(bk.reshape(b, t, B_HEADS, B_QK_DIM), positions) * (B_QK_DIM ** -0.5)
    r = head_group_norm(retention(q, k, bv.reshape(b, t, B_HEADS, B_V_DIM)))
    r = r.reshape(b, t, B_WIDTH) * jax.nn.silu(bg)
    return jnp.concatenate([a, r], axis=-1) @ w_out


def odd_mixer(h, positions, w_in, w_out, lam_params, subln, q_norm, kv_norm, w_uq, w_ukv, layer):
    b, t, _ = h.shape
    (cq, ck, cv, cg, dcq, dckv, dkr, dg) = split_columns(h @ w_in, ODD_WIDTHS)
    q = cq.reshape(b, t, C_HEADS, 2 * C_QK_DIM)
    k = ck.reshape(b, t, C_HEADS, 2 * C_QK_DIM)
    v = cv.reshape(b, t, C_HEADS, C_V_DIM)
    qs = jnp.concatenate([q[..., :C_QK_DIM], q[..., C_QK_DIM:]], axis=2)
    ks = jnp.concatenate([k[..., :C_QK_DIM], k[..., C_QK_DIM:]], axis=2)
    o = causal_attention(qs, ks, jnp.concatenate([v, v], axis=2), C_QK_DIM ** -0.5)
    lam_init = 0.8 - 0.6 * math.exp(-0.3 * layer)
    lam = (jnp.exp(jnp.sum(lam_params[0] * lam_params[1]))
           - jnp.exp(jnp.sum(lam_params[2] * lam_params[3])) + lam_init)
    c = rms_norm(o[:, :, :C_HEADS] - lam * o[:, :, C_HEADS:], subln) * (1.0 - lam_init)
    c = c.reshape(b, t, C_WIDTH) * jax.nn.silu(cg)
    qd = (rms_norm(dcq, q_norm) @ w_uq).reshape(b, t, D_HEADS, D_NOPE_DIM + D_ROPE_DIM)
    kv = (rms_norm(dckv, kv_norm) @ w_ukv).reshape(b, t, D_HEADS, D_NOPE_DIM + D_V_DIM)
    q_full = jnp.concatenate([qd[..., :D_NOPE_DIM], rotary(qd[..., D_NOPE_DIM:], positions)], axis=-1)
    k_rope = jnp.broadcast_to(rotary(dkr[:, :, None, :], positions), (b, t, D_HEADS, D_ROPE_DIM))
    k_full = jnp.concatenate([kv[..., :D_NOPE_DIM], k_rope], axis=-1)
    m = causal_attention(q_full, k_full, kv[..., D_NOPE_DIM:], (D_NOPE_DIM + D_ROPE_DIM) ** -0.5)
    m = m.reshape(b, t, D_WIDTH) * jax.nn.silu(dg)
    return jnp.concatenate([c, m], axis=-1) @ w_out


def setup_inputs(seed: int = 0) -> dict:
    key = jax.random.key(seed)
    ks = jax.random.split(key, 16)
    n_even = (DEPTH + 1) // 2
    n_odd = DEPTH // 2
    nrm = jax.random.normal
    f32 = jnp.float32
    x = nrm(ks[0], (BATCH, SEQ, D_MODEL), f32)
    offset = jax.random.randint(ks[1], (BATCH, 1), 0, MAX_POS_OFFSET)
    positions = (offset + jnp.arange(SEQ)[None, :]).astype(jnp.int32)
    return {
        'x': x,
        'positions': positions,
        'pre_norm': 1.0 + 0.02 * nrm(ks[2], (DEPTH, D_MODEL), f32),
        'post_norm': 1.0 + 0.02 * nrm(ks[3], (DEPTH, D_MODEL), f32),
        'w_in_even': nrm(ks[4], (n_even, D_MODEL, EVEN_IN), f32) * D_MODEL ** -0.5,
        'w_out_even': nrm(ks[5], (n_even, EVEN_MIX_WIDTH, D_MODEL), f32) * EVEN_MIX_WIDTH ** -0.5,
        'w_in_odd': nrm(ks[6], (n_odd, D_MODEL, ODD_IN), f32) * D_MODEL ** -0.5,
        'diff_lambda': 0.1 * nrm(ks[7], (n_odd, 4, C_QK_DIM), f32),
        'diff_subln': 1.0 + 0.02 * nrm(ks[8], (n_odd, C_V_DIM), f32),
        'mla_q_norm': 1.0 + 0.02 * nrm(ks[9], (n_odd, D_Q_LORA), f32),
        'mla_kv_norm': 1.0 + 0.02 * nrm(ks[10], (n_odd, D_KV_LORA), f32),
        'mla_w_uq': nrm(ks[11], (n_odd, D_Q_LORA, D_HEADS * (D_NOPE_DIM + D_ROPE_DIM)), f32) * D_Q_LORA ** -0.5,
        'mla_w_ukv': nrm(ks[12], (n_odd, D_KV_LORA, D_HEADS * (D_NOPE_DIM + D_V_DIM)), f32) * D_KV_LORA ** -0.5,
        'w_out_odd': nrm(ks[13], (n_odd, ODD_MIX_WIDTH, D_MODEL), f32) * ODD_MIX_WIDTH ** -0.5,
    }


def reference(x, positions, pre_norm, post_norm, w_in_even, w_out_even, w_in_odd,
              diff_lambda, diff_subln, mla_q_norm, mla_kv_norm, mla_w_uq, mla_w_ukv, w_out_odd):
    for layer in range(DEPTH):
        h = rms_norm(x, pre_norm[layer])
        j = layer // 2
        if layer % 2 == 0:
            m = even_mixer(h, positions, w_in_even[j], w_out_even[j])
        else:
            m = odd_mixer(h, positions, w_in_odd[j], w_out_odd[j], diff_lambda[j], diff_subln[j],
                          mla_q_norm[j], mla_kv_norm[j], mla_w_uq[j], mla_w_ukv[j], layer)
        x = x + rms_norm(m, post_norm[layer])
    return x
```

```python
import math
from contextlib import ExitStack

import numpy as np
import ml_dtypes

import concourse.bass as bass
import concourse.mybir as mybir
from concourse.bass_utils import run_bass_kernel_spmd

F32 = mybir.dt.float32
BF16 = mybir.dt.bfloat16
I32 = mybir.dt.int32
ALU = mybir.AluOpType
AF = mybir.ActivationFunctionType
AX = mybir.AxisListType

NCORES = 8
B, T, D = 2, 8192, 1024
TS = T // 4
EPS = 1e-6


class Buf:
    __slots__ = ("name", "w", "r", "sem", "n", "key", "excl")

    def __init__(self, name, excl=False):
        self.name = name
        self.excl = excl
        self.w = None
        self.r = {}
        self.sem = None
        self.n = 0


class Eng:
    def __init__(self, name, e, sem):
        self.name = name
        self.e = e
        self.sem = sem
        self.n = 0
        self.waited = {}


class Ctx:
    def __init__(self, nc, es):
        self.nc = nc
        self.es = es
        self.semh = {}
        self.pe = self._eng("pe", nc.tensor)
        self.dve = self._eng("dve", nc.vector)
        self.act = self._eng("act", nc.scalar)
        self.pool = self._eng("pool", nc.gpsimd)
        self.sp = self._eng("sp", nc.sync)
        self.nbuf = 0
        self.dmabufs = []
        self.sempool = []
        self.root = es

    def _eng(self, name, e):
        sem = self.es.enter_context(self.nc.semaphore("sem_" + name))
        self.semh[name] = sem
        return Eng(name, e, sem)

    def sbuf(self, name, shape, dt):
        self.nbuf += 1
        return self.es.enter_context(self.nc.sbuf_tensor("sb%d_%s" % (self.nbuf, name), list(shape), dt))

    def psum(self, name, shape, dt):
        self.nbuf += 1
        return self.es.enter_context(self.nc.psum_tensor("ps%d_%s" % (self.nbuf, name), list(shape), dt))

    def buf(self, name):
        self.nbuf += 1
        return Buf("%s_%d" % (name, self.nbuf))

    def pbuf(self, name):
        self.nbuf += 1
        return Buf("%s_%d" % (name, self.nbuf), excl=True)

    @staticmethod
    def _split(reads, writes):
        ex = [b for b in reads if b.excl]
        if ex:
            reads = [b for b in reads if not b.excl]
            writes = list(writes) + ex
        return reads, writes

    def _collect(self, reads, writes):
        deps = {}

        def add(k, v):
            if deps.get(k, 0) < v:
                deps[k] = v

        for b in reads:
            if b.w is not None:
                add(*b.w)
        for b in writes:
            if b.w is not None:
                add(*b.w)
            for k, v in b.r.items():
                add(k, v)
        return deps

    def _wait(self, eng, deps):
        for k, v in deps.items():
            if eng.waited.get(k, 0) < v:
                eng.e.wait_ge(self.semh[k], v)
                eng.waited[k] = v

    def op(self, eng, fn, reads=(), writes=()):
        reads, writes = self._split(reads, writes)
        self._wait(eng, self._collect(reads, writes))
        ins = fn(eng.e)
        eng.n += 1
        ins.then_inc(eng.sem, 1)
        for b in reads:
            b.r[eng.name] = eng.n
        for b in writes:
            b.w = (eng.name, eng.n)
            b.r = {}
        return ins

    def mm(self, fns, reads=(), writes=()):
        eng = self.pe
        reads, writes = self._split(reads, writes)
        deps = self._collect(reads, writes)
        deps.pop("pe", None)
        self._wait(eng, deps)
        ins = None
        for fn in fns:
            ins = fn(eng.e)
        eng.n += 1
        ins.then_inc(eng.sem, 1)
        for b in reads:
            b.r[eng.name] = eng.n
        for b in writes:
            b.w = (eng.name, eng.n)
            b.r = {}

    def dma(self, q, out, in_, reads=(), writes=(), owner=None, **kw):
        if owner is None:
            owner = writes[0] if writes else reads[0]
        if owner.sem is None:
            if self.sempool:
                owner.sem, owner.key, owner.n = self.sempool.pop()
            else:
                owner.key = "dmasem%d" % len(self.semh)
                owner.sem = self.root.enter_context(self.nc.semaphore(owner.key))
                self.semh[owner.key] = owner.sem
                owner.n = 0
            self.dmabufs.append(owner)
        key = owner.key
        self._wait(q, self._collect(reads, writes))
        ins = q.e.dma_start(out=out, in_=in_, **kw)
        owner.n += 16
        ins.then_inc(owner.sem, 16)
        for b in reads:
            b.r[key] = owner.n
        for b in writes:
            b.w = (key, owner.n)
            b.r = {}
        return ins

    def barrier(self):
        engs = [self.pe, self.dve, self.act, self.pool, self.sp]
        deps = {e.name: e.n for e in engs if e.n > 0}
        for b in self.dmabufs:
            deps[b.key] = b.n
        for e in engs:
            self._wait(e, dict(deps))

    def phase(self):
        return _Phase(self)

    def finish(self, bufs):
        deps = self._collect((), bufs)
        self._wait(self.sp, deps)


class _Phase:
    def __init__(self, cx):
        self.cx = cx

    def __enter__(self):
        self.saved = self.cx.es
        self.ndma = len(self.cx.dmabufs)
        self.stack = ExitStack()
        self.stack.__enter__()
        self.cx.es = self.stack
        return self

    def __exit__(self, *a):
        self.cx.barrier()
        for b in self.cx.dmabufs[self.ndma:]:
            self.cx.sempool.append((b.sem, b.key, b.n))
            b.sem = None
        del self.cx.dmabufs[self.ndma:]
        self.cx.es = self.saved
        return self.stack.__exit__(*a)


def _consts(cx):
    nc = cx.nc
    ident = cx.sbuf("ident", [128, 128], BF16)
    b = cx.buf("ident")
    tmp = cx.sbuf("ident_f", [128, 128], F32)
    tb = cx.buf("identf")
    cx.op(cx.pool, lambda e: e.memset(tmp[:], 1.0), writes=[tb])
    cx.op(cx.pool, lambda e: e.affine_select(out=tmp[:], in_=tmp[:], pattern=[[-1, 128]],
                                             compare_op=ALU.is_equal, fill=0.0, base=0,
                                             channel_multiplier=1), reads=[tb], writes=[tb])
    cx.op(cx.dve, lambda e: e.tensor_copy(out=ident[:], in_=tmp[:]), reads=[tb], writes=[b])
    return ident, b


def _tri_masks(cx):
    tk = cx.sbuf("tri_kq_f", [128, 128], F32); tkb = cx.buf("tri_kq_f")
    cx.op(cx.pool, lambda e: e.memset(tk[:], 1.0), writes=[tkb])
    cx.op(cx.pool, lambda e: e.affine_select(out=tk[:], in_=tk[:], pattern=[[1, 128]], compare_op=ALU.is_ge,
                                             fill=0.0, base=0, channel_multiplier=-1), reads=[tkb], writes=[tkb])
    tri_kq = cx.sbuf("tri_kq", [128, 128], BF16); tri_kqb = cx.buf("tri_kq")
    cx.op(cx.dve, lambda e: e.tensor_copy(out=tri_kq[:], in_=tk[:]), reads=[tkb], writes=[tri_kqb])
    tq = cx.sbuf("tri_qk", [128, 128], F32); tqb = cx.buf("tri_qk")
    cx.op(cx.pool, lambda e: e.memset(tq[:], 1.0), writes=[tqb])
    cx.op(cx.pool, lambda e: e.affine_select(out=tq[:], in_=tq[:], pattern=[[-1, 128]], compare_op=ALU.is_ge,
                                             fill=0.0, base=0, channel_multiplier=1), reads=[tqb], writes=[tqb])
    ng = cx.sbuf("neg_qk", [128, 128], F32); ngb = cx.buf("neg_qk")
    cx.op(cx.dve, lambda e: e.tensor_scalar(out=ng[:], in0=tq[:], scalar1=-1.0, scalar2=1e30, op0=ALU.add, op1=ALU.mult),
          reads=[tqb], writes=[ngb])
    nb = cx.sbuf("negb_qk", [128, 128], BF16); nbb = cx.buf("negb_qk")
    cx.op(cx.dve, lambda e: e.tensor_scalar(out=nb[:], in0=tq[:], scalar1=-1.0, scalar2=-MASK_NEG, op0=ALU.add, op1=ALU.mult),
          reads=[tqb], writes=[nbb])
    return (nb, nbb), (tq, tqb), (ng, ngb)


def _bcast_rows(ap_1d, nparts):
    return ap_1d.rearrange("(o n) -> o n", o=1).to_broadcast([nparts, ap_1d.shape[0]])


class NormT:
    def __init__(self, cx, ident, identb, gain_sb, gainb, tag):
        self.cx = cx
        self.ident, self.identb = ident, identb
        self.gain, self.gainb = gain_sb, gainb
        self.sq = cx.sbuf(tag + "_sq", [128, D], BF16)
        self.sqb = cx.buf(tag + "sq")
        self.ss = [cx.sbuf(tag + "_ss%d" % i, [128, 1], F32) for i in range(2)]
        self.ssb = [cx.buf(tag + "ss") for i in range(2)]
        self.h = [cx.sbuf(tag + "_h%d" % i, [128, D], BF16) for i in range(2)]
        self.hb = [cx.buf(tag + "h") for i in range(2)]
        self.tp = [cx.psum(tag + "_tp%d" % i, [128, 8, 128], BF16) for i in range(2)]
        self.tpb = [cx.pbuf(tag + "tp") for i in range(2)]
        self.st = [cx.sbuf(tag + "_st%d" % i, [128, 8, 512], BF16) for i in range(2)]
        self.stb = [cx.buf(tag + "st") for i in range(2)]
        self.i = 0

    def tile(self, x_ap, xb, hT_dram, tok0):
        cx = self.cx
        i = self.i
        self.i += 1
        p = i % 2
        s = (i // 4) % 2
        sub = i % 4
        ss, ssb = self.ss[p], self.ssb[p]
        cx.op(cx.act, lambda e: e.activation(out=self.sq[:], in_=x_ap, func=AF.Square,
                                             accum_out=ss[:]), reads=[xb], writes=[self.sqb, ssb])
        cx.op(cx.dve, lambda e: e.tensor_scalar(out=ss[:], in0=ss[:], scalar1=1.0 / D, scalar2=EPS,
                                                op0=ALU.mult, op1=ALU.add), reads=[ssb], writes=[ssb])
        cx.op(cx.act, lambda e: e.activation(out=ss[:], in_=ss[:], func=AF.Sqrt), reads=[ssb], writes=[ssb])
        cx.op(cx.dve, lambda e: e.reciprocal(out=ss[:], in_=ss[:]), reads=[ssb], writes=[ssb])
        h, hb = self.h[p], self.hb[p]
        cx.op(cx.dve, lambda e: e.scalar_tensor_tensor(out=h[:], in0=x_ap, scalar=ss[:, 0:1], in1=self.gain[:],
                                                       op0=ALU.mult, op1=ALU.mult),
              reads=[xb, ssb, self.gainb], writes=[hb])
        tp, tpb = self.tp[p], self.tpb[p]
        cx.mm([(lambda e, c=c: e.transpose(out=tp[:, c, :], in_=h[:, c * 128:(c + 1) * 128], identity=self.ident[:]))
               for c in range(8)], reads=[hb, self.identb], writes=[tpb])
        st, stb = self.st[s], self.stb[s]
        cx.op(cx.act, lambda e: e.copy(out=st[:, :, sub * 128:(sub + 1) * 128], in_=tp[:]),
              reads=[tpb], writes=[stb])
        if sub == 3:
            t0 = tok0 - 384
            dst = hT_dram.rearrange("(c p) t -> p c t", p=128)[:, :, t0:t0 + 512]
            cx.dma(cx.sp, dst, st[:], reads=[stb], writes=[])


def build_p0():
    nc = bass.Bass("TRN2", target_bir_lowering=False)
    x = nc.dram_tensor("x", [TS, D], F32, kind="ExternalInput").ap()
    g = nc.dram_tensor("g", [D], F32, kind="ExternalInput").ap()
    hT = nc.dram_tensor("hT", [D, TS], BF16, kind="ExternalOutput").ap()
    with ExitStack() as es:
        cx = Ctx(nc, es)
        ident, identb = _consts(cx)
        gain = cx.sbuf("gain", [128, D], F32)
        gainb = cx.buf("gain")
        cx.dma(cx.sp, gain[:], _bcast_rows(g, 128), writes=[gainb])
        nt = NormT(cx, ident, identb, gain, gainb, "n0")
        xs = [cx.sbuf("x%d" % i, [128, D], F32) for i in range(2)]
        xb = [cx.buf("x") for i in range(2)]
        for i in range(TS // 128):
            p = i % 2
            cx.dma(cx.sp, xs[p][:], x[i * 128:(i + 1) * 128, :], writes=[xb[p]])
            nt.tile(xs[p][:], xb[p], hT, i * 128)
        cx.finish(nt.stb)
    return nc


def load_weight_bf16(cx, w_dram, rows, cols, name, q=None, col0=0, stage=None):
    nch = rows // 128
    w = cx.sbuf(name, [128, nch, cols], BF16)
    wb = cx.buf(name)
    if stage is None:
        stage = ([cx.sbuf(name + "_stg%d" % i, [128, cols], F32) for i in range(2)],
                 [cx.buf(name + "stg") for i in range(2)])
    sts, stb = stage
    for c in range(nch):
        p = c % 2
        cx.dma(q or cx.sp, sts[p][:, :cols], w_dram[c * 128:(c + 1) * 128, col0:col0 + cols], writes=[stb[p]])
        cx.op(cx.pool, lambda e, c=c, p=p: e.tensor_copy(out=w[:, c, :], in_=sts[p][:, :cols]),
              reads=[stb[p]], writes=[wb])
    return w, wb


def build_p3(last):
    nc = bass.Bass("TRN2", target_bir_lowering=False)
    x = nc.dram_tensor("x", [TS, D], F32, kind="ExternalInput").ap()
    y = nc.dram_tensor("y", [TS, D], BF16, kind="ExternalInput").ap()
    wo = nc.dram_tensor("wo", [D, D], F32, kind="ExternalInput").ap()
    gp = nc.dram_tensor("gp", [D], F32, kind="ExternalInput").ap()
    xo = nc.dram_tensor("xo", [TS, D], F32, kind="ExternalOutput").ap()
    if not last:
        gn = nc.dram_tensor("gn", [D], F32, kind="ExternalInput").ap()
        hT = nc.dram_tensor("hT", [D, TS], BF16, kind="ExternalOutput").ap()
    with ExitStack() as es:
        cx = Ctx(nc, es)
        p3_body(cx, x, y, wo, gp, xo, None if last else gn, None if last else hT)
    return nc


def p3_body(cx, x, y, wo, gp, xo, gn, hT):
    ident, identb = _consts(cx)
    gpost = cx.sbuf("gpost", [128, D], F32)
    gpostb = cx.buf("gpost")
    cx.dma(cx.sp, gpost[:], _bcast_rows(gp, 128), writes=[gpostb])
    nt = None
    if gn is not None:
        gnext = cx.sbuf("gnext", [128, D], F32)
        gnextb = cx.buf("gnext")
        cx.dma(cx.sp, gnext[:], _bcast_rows(gn, 128), writes=[gnextb])
        nt = NormT(cx, ident, identb, gnext, gnextb, "n3")
    w, wb = load_weight_bf16(cx, wo, D, D, "wout")
    xs = [cx.sbuf("x%d" % i, [128, D], F32) for i in range(2)]
    xb = [cx.buf("x") for i in range(2)]
    ys = [cx.sbuf("y%d" % i, [128, D], BF16) for i in range(2)]
    yb = [cx.buf("y") for i in range(2)]
    yT = [cx.sbuf("yT%d" % i, [128, 8, 128], BF16) for i in range(2)]
    yTb = [cx.buf("yT") for i in range(2)]
    tp = cx.psum("p3tp", [128, 8, 128], BF16)
    tpb = cx.pbuf("p3tp")
    mps = [cx.psum("p3m%d" % i, [128, 512], F32) for i in range(4)]
    mpb = [cx.pbuf("p3m") for i in range(4)]
    ss = [cx.sbuf("p3ss%d" % i, [128, 2], F32) for i in range(2)]
    ssb = [cx.buf("p3ss") for i in range(2)]
    junk = cx.sbuf("p3junk", [128, 512], BF16)
    junkb = cx.buf("p3junk")
    x1 = [cx.sbuf("x1_%d" % i, [128, D], F32) for i in range(2)]
    x1b = [cx.buf("x1") for i in range(2)]
    for i in range(TS // 128):
        p = i % 2
        cx.dma(cx.sp, xs[p][:], x[i * 128:(i + 1) * 128, :], writes=[xb[p]])
        cx.dma(cx.sp, ys[p][:], y[i * 128:(i + 1) * 128, :], writes=[yb[p]])
        cx.mm([(lambda e, c=c: e.transpose(out=tp[:, c, :], in_=ys[p][:, c * 128:(c + 1) * 128], identity=ident[:]))
               for c in range(8)], reads=[yb[p], identb], writes=[tpb])
        cx.op(cx.act, lambda e: e.copy(out=yT[p][:], in_=tp[:]), reads=[tpb], writes=[yTb[p]])
        for hh in range(2):
            m, mb = mps[2 * p + hh], mpb[2 * p + hh]
            cx.mm([(lambda e, c=c: e.matmul(out=m[:], lhsT=yT[p][:, c, :], rhs=w[:, c, hh * 512:(hh + 1) * 512],
                                            start=(c == 0), stop=(c == 7))) for c in range(8)],
                  reads=[yTb[p], wb], writes=[mb])
            cx.op(cx.act, lambda e: e.activation(out=junk[:], in_=m[:], func=AF.Square, accum_out=ss[p][:, hh:hh + 1]),
                  reads=[mb], writes=[junkb, ssb[p]])
        s = ss[p]
        cx.op(cx.dve, lambda e: e.tensor_tensor(out=s[:, 0:1], in0=s[:, 0:1], in1=s[:, 1:2], op=ALU.add),
              reads=[ssb[p]], writes=[ssb[p]])
        cx.op(cx.dve, lambda e: e.tensor_scalar(out=s[:, 0:1], in0=s[:, 0:1], scalar1=1.0 / D, scalar2=EPS,
                                                op0=ALU.mult, op1=ALU.add), reads=[ssb[p]], writes=[ssb[p]])
        cx.op(cx.act, lambda e: e.activation(out=s[:, 0:1], in_=s[:, 0:1], func=AF.Sqrt), reads=[ssb[p]], writes=[ssb[p]])
        cx.op(cx.dve, lambda e: e.reciprocal(out=s[:, 0:1], in_=s[:, 0:1]), reads=[ssb[p]], writes=[ssb[p]])
        for hh in range(2):
            m, mb = mps[2 * p + hh], mpb[2 * p + hh]
            cx.op(cx.dve, lambda e: e.scalar_tensor_tensor(out=x1[p][:, hh * 512:(hh + 1) * 512], in0=m[:],
                                                           scalar=s[:, 0:1], in1=gpost[:, hh * 512:(hh + 1) * 512],
                                                           op0=ALU.mult, op1=ALU.mult),
                  reads=[mb, ssb[p], gpostb], writes=[x1b[p]])
        cx.op(cx.pool, lambda e: e.tensor_tensor(out=x1[p][:], in0=x1[p][:], in1=xs[p][:], op=ALU.add),
              reads=[xb[p], x1b[p]], writes=[x1b[p]])
        cx.dma(cx.sp, xo[i * 128:(i + 1) * 128, :], x1[p][:], reads=[x1b[p]], writes=[])
        if nt is not None:
            nt.tile(x1[p][:], x1b[p], hT, i * 128)
    cx.finish(x1b + (nt.stb if nt is not None else []))


PI = math.pi
MASK_NEG = -30000.0
TWO_PI = 2.0 * math.pi
PI_LO = 3.1415925
NCH = T // 512
NBLK = T // 128


class HStream:
    def __init__(self, cx, hT):
        self.cx = cx
        self.src = hT.rearrange("(c p) t -> p c t", p=128)
        self.t = [cx.sbuf("hTc%d" % i, [128, 8, 512], BF16) for i in range(2)]
        self.b = [cx.buf("hTc") for i in range(2)]
        self.i = 0

    def load(self, ci):
        p = self.i % 2
        self.i += 1
        self.cx.dma(self.cx.sp, self.t[p][:], self.src[:, :, ci * 512:(ci + 1) * 512], writes=[self.b[p]])
        return self.t[p], self.b[p]


def proj_T(cx, ps_ap, psb, w, wb, col0, m, hc, hcb):
    cx.mm([(lambda e, c=c: e.matmul(out=ps_ap, lhsT=w[:, c, col0:col0 + m], rhs=hc[:, c, :],
                                    start=(c == 0), stop=(c == 7))) for c in range(8)],
          reads=[wb, hcb], writes=[psb])


def proj_tok(cx, ps_ap, psb, w, wb, col0, n, hc, hcb, sub):
    cx.mm([(lambda e, c=c: e.matmul(out=ps_ap, lhsT=hc[:, c, sub * 128:(sub + 1) * 128], rhs=w[:, c, col0:col0 + n],
                                    start=(c == 0), stop=(c == 7))) for c in range(8)],
          reads=[wb, hcb], writes=[psb])


class Rope:
    def __init__(self, cx, pos_dram, rc, rcb, lo, hi):
        self.cx = cx
        self.pos = pos_dram
        self.rc, self.rcb = rc, rcb
        self.lo, self.hi = lo, hi
        self.posi = cx.sbuf("rp_posi", [128, 512], I32)
        self.posib = cx.buf("rp_posi")
        self.ang = cx.sbuf("rp_ang", [128, 512], F32)
        self.angb = cx.buf("rp_ang")
        self.a = cx.sbuf("rp_a", [128, 512], F32)
        self.ab = cx.buf("rp_a")
        self.ki = cx.sbuf("rp_ki", [128, 512], I32)
        self.kib = cx.buf("rp_ki")
        self.m = cx.sbuf("rp_m", [128, 512], F32)
        self.mb = cx.buf("rp_m")
        self.cos = cx.sbuf("rp_cos", [128, 512], F32)
        self.cosb = cx.buf("rp_cos")
        self.sin = cx.sbuf("rp_sin", [128, 512], F32)
        self.sinb = cx.buf("rp_sin")

    def compute(self, ci):
        cx = self.cx
        s = slice(self.lo, self.hi)
        n = self.hi - self.lo
        src = self.pos[ci * 512:(ci + 1) * 512].rearrange("(o n) -> o n", o=1).to_broadcast([n, 512])
        cx.dma(cx.sp, self.posi[s, :], src, writes=[self.posib])
        cx.op(cx.dve, lambda e: e.tensor_scalar(out=self.ang[s, :], in0=self.posi[s, :], scalar1=self.rc[s, 0:1],
                                                scalar2=None, op0=ALU.mult),
              reads=[self.posib, self.rcb], writes=[self.angb])
        for shift, out, outb, signed in ((0.0, self.sin, self.sinb, True), (PI / 2, self.cos, self.cosb, False)):
            a, ab, ki, kib, m, mb = self.a, self.ab, self.ki, self.kib, self.m, self.mb
            cx.op(cx.dve, lambda e: e.tensor_scalar(out=a[s, :], in0=self.ang[s, :], scalar1=shift, scalar2=None,
                                                    op0=ALU.add), reads=[self.angb], writes=[ab])
            cx.op(cx.dve, lambda e: e.tensor_scalar(out=ki[s, :], in0=a[s, :], scalar1=1.0 / TWO_PI, scalar2=None,
                                                    op0=ALU.mult), reads=[ab], writes=[kib])
            cx.op(cx.dve, lambda e: e.scalar_tensor_tensor(out=a[s, :], in0=ki[s, :], scalar=-TWO_PI, in1=a[s, :],
                                                           op0=ALU.mult, op1=ALU.add), reads=[kib, ab], writes=[ab])
            cx.op(cx.dve, lambda e: e.tensor_scalar(out=m[s, :], in0=a[s, :], scalar1=PI, scalar2=-TWO_PI,
                                                    op0=ALU.is_gt, op1=ALU.mult), reads=[ab], writes=[mb])
            cx.op(cx.dve, lambda e: e.tensor_tensor(out=a[s, :], in0=a[s, :], in1=m[s, :], op=ALU.add),
                  reads=[ab, mb], writes=[ab])
            cx.op(cx.dve, lambda e: e.tensor_scalar(out=a[s, :], in0=a[s, :], scalar1=PI_LO, scalar2=-PI_LO,
                                                    op0=ALU.min, op1=ALU.max), reads=[ab], writes=[ab])
            if signed:
                cx.op(cx.act, lambda e: e.activation(out=out[s, :], in_=a[s, :], func=AF.Sin, scale=self.rc[s, 1:2]),
                      reads=[ab, self.rcb], writes=[outb])
            else:
                cx.op(cx.act, lambda e: e.activation(out=out[s, :], in_=a[s, :], func=AF.Sin),
                      reads=[ab], writes=[outb])

    def apply(self, dst_ap, dstb, x_ps, xb, xp_ps, xpb, tmp, tmpb, scale=1.0):
        cx = self.cx
        s = slice(self.lo, self.hi)
        t1, t2 = tmp
        cx.op(cx.dve, lambda e: e.tensor_tensor(out=t1[s, :], in0=x_ps, in1=self.cos[s, :], op=ALU.mult),
              reads=[xb, self.cosb], writes=[tmpb[0]])
        cx.op(cx.dve, lambda e: e.tensor_tensor(out=t2[s, :], in0=xp_ps, in1=self.sin[s, :], op=ALU.mult),
              reads=[xpb, self.sinb], writes=[tmpb[1]])
        for d, db in zip(dst_ap, dstb):
            if scale == 1.0:
                cx.op(cx.pool, lambda e, d=d: e.tensor_tensor(out=d, in0=t1[s, :], in1=t2[s, :], op=ALU.add),
                      reads=[tmpb[0], tmpb[1]], writes=[db])
            else:
                cx.op(cx.dve, lambda e, d=d: e.scalar_tensor_tensor(out=d, in0=t1[s, :], scalar=scale, in1=t2[s, :],
                                                                     op0=ALU.mult, op1=ALU.add),
                      reads=[tmpb[0], tmpb[1]], writes=[db])


class Stream:
    def __init__(self, qt, qtb, kt, ktb, v, vb, ncol, accs, accb, first_of_bank):
        self.qt, self.qtb = qt, qtb
        self.kt, self.ktb = kt, ktb
        self.v, self.vb = v, vb
        self.ncol = ncol
        self.accs, self.accb = accs, accb
        self.first = first_of_bank


def causal_attention(cx, streams, scale, epilogue, s_ps, s_psb, pts, ptb, extra_mask=None, qchunks=None,
                     qw=4, diag_mask=None, prologue=None):
    ns = len(streams)
    cnt = [0] * ns
    W = qw * 128

    def issue_s(qc, kb):
        j0 = max(0, kb - qw * qc)
        for si, st in enumerate(streams):
            p = cnt[si] % 2
            cols = slice(j0 * 128, W)
            diag = diag_mask is not None and kb >= qw * qc
            fns = [lambda e, st=st, p=p, cols=cols, si=si: e.matmul(out=s_ps[si][p][:, cols], lhsT=st.kt(kb),
                                                                    rhs=st.qt(qc)[:, cols], start=True,
                                                                    stop=(extra_mask is None and not diag))]
            rd = [st.ktb, st.qtb]
            if diag:
                dg = slice(j0 * 128, (j0 + 1) * 128)
                (ng, ngb), (idn, idnb) = diag_mask
                fns.append(lambda e, p=p, si=si, dg=dg: e.matmul(out=s_ps[si][p][:, dg], lhsT=ng[:], rhs=idn[:],
                                                                 start=False, stop=True))
                rd += [ngb, idnb]
            if extra_mask is not None:
                mfn, mrd = extra_mask(qc, kb, s_ps[si][p])
                fns += mfn
                rd += mrd
            cx.mm(fns, reads=rd, writes=[s_psb[si][p]])

    for qc in (qchunks if qchunks is not None else range(NBLK // qw)):
        if prologue is not None:
            prologue(qc)
        nkb = qw * (qc + 1)
        issue_s(qc, 0)
        for kb in range(nkb):
            j0 = max(0, kb - qw * qc)
            cols = slice(j0 * 128, W)
            cur = [cnt[si] % 2 for si in range(ns)]
            for si in range(ns):
                cnt[si] += 1
            for si, st in enumerate(streams):
                p = cur[si]
                cx.op(cx.act, lambda e, si=si, p=p: e.activation(out=pts[si][p][:, cols], in_=s_ps[si][p][:, cols],
                                                                   func=AF.Exp, scale=scale),
                      reads=[s_psb[si][p]], writes=[ptb[si][p]])
            if kb + 1 < nkb:
                issue_s(qc, kb + 1)
            for si, st in enumerate(streams):
                p = cur[si]
                for j in range(j0, qw if 'nopv' not in DBG else 0):
                    first = (kb == 0)
                    cx.mm([lambda e, st=st, p=p, j=j, si=si: e.matmul(
                        out=st.accs[j], lhsT=pts[si][p][:, j * 128:(j + 1) * 128], rhs=st.v(kb),
                        start=(first and st.first[j]), stop=(kb == qw * qc + j), skip_group_check=True)],
                        reads=[ptb[si][p], st.vb], writes=[st.accb[j]])
        if 'noepi' not in DBG:
            epilogue(qc)


def _load_small(cx, dram_ap, shape, name, dt=F32):
    t = cx.sbuf(name, shape, dt)
    b = cx.buf(name)
    cx.dma(cx.sp, t[:], dram_ap, writes=[b])
    return t, b


def silu_ps(cx, out_ap, outb, x_ap, xb, tmp, tmpb):
    n = x_ap.shape[-1]
    t = tmp[:, 0:n]
    cx.op(cx.act, lambda e: e.activation(out=t, in_=x_ap, func=AF.Exp, scale=-1.0), reads=[xb], writes=[tmpb])
    cx.op(cx.pool, lambda e: e.tensor_scalar(out=t, in0=t, scalar1=1.0, scalar2=None, op0=ALU.add),
          reads=[tmpb], writes=[tmpb])
    cx.op(cx.dve, lambda e: e.reciprocal(out=t, in_=t), reads=[tmpb], writes=[tmpb])
    cx.op(cx.dve, lambda e: e.tensor_tensor(out=out_ap, in0=x_ap, in1=t, op=ALU.mult), reads=[xb, tmpb], writes=[outb])


def _rstd(cx, ss_ap, ssb, n, eps_ap=None):
    if eps_ap is None:
        cx.op(cx.dve, lambda e: e.tensor_scalar(out=ss_ap, in0=ss_ap, scalar1=1.0 / n, scalar2=EPS,
                                                op0=ALU.mult, op1=ALU.add), reads=[ssb], writes=[ssb])
    else:
        cx.op(cx.dve, lambda e: e.tensor_scalar(out=ss_ap, in0=ss_ap, scalar1=1.0 / n, scalar2=eps_ap,
                                                op0=ALU.mult, op1=ALU.add), reads=[ssb], writes=[ssb])
    cx.op(cx.act, lambda e: e.activation(out=ss_ap, in_=ss_ap, func=AF.Sqrt), reads=[ssb], writes=[ssb])
    cx.op(cx.dve, lambda e: e.reciprocal(out=ss_ap, in_=ss_ap), reads=[ssb], writes=[ssb])


def mixer_c(cx, hT, wC, lamp, subln, y_out, ycol0, lam_init):
    with cx.phase():
        w, wb = load_weight_bf16(cx, wC, D, 512, "wC")
        QT = cx.sbuf("cQT", [128, T], BF16); QTb = cx.buf("cQT")
        KT = cx.sbuf("cKT", [128, T], BF16); KTb = cx.buf("cKT")
        V = cx.sbuf("cV", [128, NBLK, 129], BF16); Vb = cx.buf("cV")
        G = cx.sbuf("cG", [128, NBLK, 128], BF16); Gb = cx.buf("cG")
        cx.op(cx.pool, lambda e: e.memset(V[:, :, 128:129], 1.0), writes=[Vb])
        tri_kq, _, _ = _tri_masks(cx)
        ident, identb = _consts(cx)
        lt, ltb = _load_small(cx, lamp.rearrange("(o a) n -> o (a n)", o=1).to_broadcast([128, 256]), [128, 256], "lamp")
        sl, slb = _load_small(cx, _bcast_rows(subln, 128), [128, 128], "subln")
        lam = cx.sbuf("lam", [128, 4], F32); lamb = cx.buf("lam")
        junk = cx.sbuf("cjunk", [128, 128], F32); junkb = cx.buf("cjunk")
        for k in range(2):
            cx.op(cx.dve, lambda e, k=k: e.tensor_tensor(out=junk[:, 0:64], in0=lt[:, 128 * k:128 * k + 64],
                                                         in1=lt[:, 128 * k + 64:128 * k + 128], op=ALU.mult),
                  reads=[ltb], writes=[junkb])
            cx.op(cx.dve, lambda e, k=k: e.reduce_sum(out=lam[:, k:k + 1], in_=junk[:, 0:64], axis=AX.X),
                  reads=[junkb], writes=[lamb])
        cx.op(cx.act, lambda e: e.activation(out=lam[:, 0:2], in_=lam[:, 0:2], func=AF.Exp), reads=[lamb], writes=[lamb])
        cx.op(cx.dve, lambda e: e.tensor_tensor(out=lam[:, 2:3], in0=lam[:, 1:2], in1=lam[:, 0:1], op=ALU.subtract),
              reads=[lamb], writes=[lamb])
        cx.op(cx.dve, lambda e: e.tensor_scalar(out=lam[:, 2:3], in0=lam[:, 2:3], scalar1=-lam_init, scalar2=None,
                                                op0=ALU.add), reads=[lamb], writes=[lamb])
        cx.op(cx.dve, lambda e: e.tensor_scalar(out=sl[:], in0=sl[:], scalar1=1.0 - lam_init, scalar2=None,
                                                op0=ALU.mult), reads=[slb], writes=[slb])
        with cx.phase():
            hs = HStream(cx, hT)
            pT = [cx.psum("cpT%d" % i, [128, 512], F32) for i in range(2)]
            pTb = [cx.pbuf("cpT") for i in range(2)]
            pk = [cx.psum("cpk%d" % i, [128, 512], F32) for i in range(2)]
            pkb = [cx.pbuf("cpk") for i in range(2)]
            sg = cx.sbuf("c_sg", [128, 128], F32); sgb = cx.buf("c_sg")
            for ci in range(NCH if 'nop1' not in DBG else 0):
                hc, hcb = hs.load(ci)
                if 'noQ' not in DBG:
                    proj_T(cx, pT[0][:], pTb[0], w, wb, 0, 128, hc, hcb)
                    cx.op(cx.act, lambda e: e.copy(out=QT[:, ci * 512:(ci + 1) * 512], in_=pT[0][:]),
                          reads=[pTb[0]], writes=[QTb])
                if 'noK' not in DBG:
                    proj_T(cx, pT[1][:], pTb[1], w, wb, 128, 128, hc, hcb)
                    cx.op(cx.dve, lambda e: e.tensor_copy(out=KT[:, ci * 512:(ci + 1) * 512], in_=pT[1][:]),
                          reads=[pTb[1]], writes=[KTb])
                for sub in range(4 if 'noVG' not in DBG else 0):
                    blk = ci * 4 + sub
                    p = sub % 2
                    proj_tok(cx, pk[p][:, 0:256], pkb[p], w, wb, 256, 256, hc, hcb, sub)
                    cx.op(cx.dve, lambda e: e.tensor_copy(out=V[:, blk, 0:128], in_=pk[p][:, 0:128]),
                          reads=[pkb[p]], writes=[Vb])
                    silu_ps(cx, G[:, blk, :], Gb, pk[p][:, 128:256], pkb[p], sg, sgb)
        with cx.phase():
            s_ps = [[cx.psum("cS%d_%d" % (s, i), [128, 512], F32) for i in range(2)] for s in range(2)]
            s_psb = [[cx.pbuf("cS") for i in range(2)] for s in range(2)]
            pts = [[cx.sbuf("cP%d_%d" % (s, i), [128, 512], BF16) for i in range(2)] for s in range(2)]
            ptb = [[cx.buf("cP") for i in range(2)] for s in range(2)]
            streams = []
            for s in range(2):
                banks = [cx.psum("cO%d_%d" % (s, i), [128, 2, 256], F32) for i in range(2)]
                accs = [banks[j // 2][:, j % 2, 0:129] for j in range(4)]
                bb = [cx.pbuf("cO") for i in range(2)]
                accb = [bb[j // 2] for j in range(4)]
                rows = slice(64 * s, 64 * s + 64)
                streams.append(Stream(
                    qt=lambda qc, rows=rows: QT[rows, qc * 512:(qc + 1) * 512], qtb=QTb,
                    kt=lambda kb, rows=rows: KT[rows, kb * 128:(kb + 1) * 128], ktb=KTb,
                    v=lambda kb: V[:, kb, :], vb=Vb, ncol=129, accs=accs, accb=accb,
                    first_of_bank=[True, False, True, False]))
            rr = cx.sbuf("c_rr", [128, 4], F32); rrb = cx.buf("c_rr")
            t1 = cx.sbuf("c_t1", [128, 128], F32); t1b = cx.buf("c_t1")
            dd = cx.sbuf("c_dd", [128, 128], F32); ddb = cx.buf("c_dd")
            yy = [cx.sbuf("c_yy%d" % i, [128, 128], BF16) for i in range(2)]
            yyb = [cx.buf("c_yy") for i in range(2)]

            def epilogue(qc):
                for j in range(4):
                    blk = qc * 4 + j
                    a1, a2 = streams[0].accs[j], streams[1].accs[j]
                    b1, b2 = streams[0].accb[j], streams[1].accb[j]
                    cx.op(cx.dve, lambda e: e.reciprocal(out=rr[:, 0:1], in_=a1[:, 128:129]), reads=[b1], writes=[rrb])
                    cx.op(cx.dve, lambda e: e.reciprocal(out=rr[:, 1:2], in_=a2[:, 128:129]), reads=[b2], writes=[rrb])
                    cx.op(cx.dve, lambda e: e.tensor_tensor(out=rr[:, 1:2], in0=rr[:, 1:2], in1=lam[:, 2:3], op=ALU.mult),
                          reads=[rrb, lamb], writes=[rrb])
                    cx.op(cx.dve, lambda e: e.tensor_scalar(out=t1[:], in0=a1[:, 0:128], scalar1=rr[:, 0:1], scalar2=None,
                                                            op0=ALU.mult), reads=[b1, rrb], writes=[t1b])
                    cx.op(cx.dve, lambda e: e.scalar_tensor_tensor(out=dd[:], in0=a2[:, 0:128], scalar=rr[:, 1:2], in1=t1[:],
                                                                   op0=ALU.mult, op1=ALU.add),
                          reads=[b2, rrb, t1b], writes=[ddb])
                    cx.op(cx.act, lambda e: e.activation(out=t1[:], in_=dd[:], func=AF.Square, accum_out=rr[:, 2:3]),
                          reads=[ddb], writes=[t1b, rrb])
                    _rstd(cx, rr[:, 2:3], rrb, 128)
                    cx.op(cx.dve, lambda e: e.scalar_tensor_tensor(out=dd[:], in0=dd[:], scalar=rr[:, 2:3], in1=sl[:],
                                                                   op0=ALU.mult, op1=ALU.mult),
                          reads=[ddb, rrb, slb], writes=[ddb])
                    p = blk % 2
                    cx.op(cx.pool, lambda e: e.tensor_tensor(out=yy[p][:], in0=dd[:], in1=G[:, blk, :], op=ALU.mult),
                          reads=[ddb, Gb], writes=[yyb[p]])
                    cx.dma(cx.sp, y_out[blk * 128:(blk + 1) * 128, ycol0:ycol0 + 128], yy[p][:], reads=[yyb[p]])

            if 'noattn' not in DBG:
                causal_attention(cx, streams, C_SCALE, epilogue, s_ps, s_psb, pts, ptb, qchunks=DBG_QCH, diag_mask=(tri_kq, (ident, identb)))


C_SCALE = 64 ** -0.5
DBG_QCH = None
DBG = set()
D_SCALE = 96 ** -0.5
LAM_INIT_L1 = 0.8 - 0.6 * math.exp(-0.3 * 1)


def mixer_d(cx, hT, pos, wD, wuq, wukv, qn, kvn, ropeD, y_out, ycol0):
    with cx.phase():
        ident, identb = _consts(cx)
        w, wb = load_weight_bf16(cx, wD, D, 576, "wD")
        wq, wqb = load_weight_bf16(cx, wuq, 256, 384, "wuq")
        wkv, wkvb = load_weight_bf16(cx, wukv, 128, 256, "wukv")
        QT = [cx.sbuf("dQT%d" % h, [96, T], BF16) for h in range(2)]
        QTb = [cx.buf("dQT") for h in range(2)]
        KT = [cx.sbuf("dKT%d" % h, [96, T], BF16) for h in range(2)]
        KTb = [cx.buf("dKT") for h in range(2)]
        V = cx.sbuf("dV", [128, NBLK, 2, 65], BF16); Vb = cx.buf("dV")
        G = cx.sbuf("dG", [128, NBLK, 128], BF16); Gb = cx.buf("dG")
        cx.op(cx.pool, lambda e: e.memset(V[:, :, :, 64:65], 1.0), writes=[Vb])
        tri_kq, _, _ = _tri_masks(cx)
        rc, rcb = _load_small(cx, ropeD, [128, 2], "ropeD")
        gq, gqb = _load_small(cx, _bcast_rows(qn, 128), [128, 256], "gq")
        gkv, gkvb = _load_small(cx, _bcast_rows(kvn, 128), [128, 128], "gkv")
        with cx.phase():
            hs = HStream(cx, hT)
            rope = Rope(cx, pos, rc, rcb, 64, 96)
            tok = [cx.psum("dtok%d" % i, [128, 512], F32) for i in range(2)]
            tokb = [cx.pbuf("dtok") for i in range(2)]
            tp = cx.psum("dtp", [128, 8, 128], BF16); tpb = cx.pbuf("dtp")
            gp = [cx.psum("dgp%d" % i, [128, 512], F32) for i in range(3)]
            gpb = [cx.pbuf("dgp") for i in range(3)]
            vp = cx.psum("dvp", [128, 512], F32); vpb = cx.pbuf("dvp")
            ss = cx.sbuf("d_ss", [128, 2], F32); ssb = cx.buf("d_ss")
            junk = cx.sbuf("d_junk", [128, 256], BF16); junkb = cx.buf("d_junk")
            cn = cx.sbuf("d_cn", [128, 384], BF16); cnb = cx.buf("d_cn")
            cT = cx.sbuf("d_cT", [128, 3, 512], BF16); cTb = cx.buf("d_cT")
            sg = cx.sbuf("d_sg", [128, 128], F32); sgb = cx.buf("d_sg")
            tmp = [cx.sbuf("d_tmp%d" % i, [128, 512], F32) for i in range(2)]
            tmpb = [cx.buf("d_tmp") for i in range(2)]
            gi = 0
            for ci in range(NCH):
                cs = slice(ci * 512, (ci + 1) * 512)
                hc, hcb = hs.load(ci)
                rope.compute(ci)
                for sub in range(4):
                    blk = ci * 4 + sub
                    p = sub % 2
                    t, tb = tok[p], tokb[p]
                    proj_tok(cx, t[:], tb, w, wb, 0, 512, hc, hcb, sub)
                    cx.op(cx.act, lambda e: e.activation(out=junk[:, 0:256], in_=t[:, 0:256], func=AF.Square,
                                                         accum_out=ss[:, 0:1]), reads=[tb], writes=[junkb, ssb])
                    cx.op(cx.act, lambda e: e.activation(out=junk[:, 0:128], in_=t[:, 256:384], func=AF.Square,
                                                         accum_out=ss[:, 1:2]), reads=[tb], writes=[junkb, ssb])
                    _rstd(cx, ss[:, 0:1], ssb, 256)
                    _rstd(cx, ss[:, 1:2], ssb, 128)
                    cx.op(cx.dve, lambda e: e.scalar_tensor_tensor(out=cn[:, 0:256], in0=t[:, 0:256], scalar=ss[:, 0:1],
                                                                   in1=gq[:], op0=ALU.mult, op1=ALU.mult),
                          reads=[tb, ssb, gqb], writes=[cnb])
                    cx.op(cx.dve, lambda e: e.scalar_tensor_tensor(out=cn[:, 256:384], in0=t[:, 256:384], scalar=ss[:, 1:2],
                                                                   in1=gkv[:], op0=ALU.mult, op1=ALU.mult),
                          reads=[tb, ssb, gkvb], writes=[cnb])
                    silu_ps(cx, G[:, blk, :], Gb, t[:, 384:512], tb, sg, sgb)
                    cx.mm([(lambda e, c=c: e.transpose(out=tp[:, c, :], in_=cn[:, c * 128:(c + 1) * 128], identity=ident[:]))
                           for c in range(3)], reads=[cnb, identb], writes=[tpb])
                    cx.op(cx.act, lambda e: e.copy(out=cT[:, :, sub * 128:(sub + 1) * 128], in_=tp[:, 0:3, :]),
                          reads=[tpb], writes=[cTb])
                for h in range(2):
                    a, ab = gp[gi % 3], gpb[gi % 3]; gi += 1
                    b2, b2b = gp[gi % 3], gpb[gi % 3]; gi += 1
                    cx.mm([(lambda e, c=c: e.matmul(out=a[0:96, :], lhsT=wq[:, c, h * 192:h * 192 + 96], rhs=cT[:, c, :],
                                                    start=(c == 0), stop=(c == 1))) for c in range(2)],
                          reads=[wqb, cTb], writes=[ab])
                    cx.mm([(lambda e, c=c: e.matmul(out=b2[0:96, :], lhsT=wq[:, c, h * 192 + 96:h * 192 + 192], rhs=cT[:, c, :],
                                                    start=(c == 0), stop=(c == 1))) for c in range(2)],
                          reads=[wqb, cTb], writes=[b2b])
                    cx.op(cx.act, lambda e: e.copy(out=QT[h][0:64, cs], in_=a[0:64, :]), reads=[ab], writes=[QTb[h]])
                    rope.apply([QT[h][64:96, cs]], [QTb[h]], a[64:96, :], ab, b2[64:96, :], b2b, tmp, tmpb)
                for h in range(2):
                    a, ab = gp[gi % 3], gpb[gi % 3]; gi += 1
                    cx.mm([lambda e: e.matmul(out=a[0:64, :], lhsT=wkv[:, 0, h * 64:(h + 1) * 64], rhs=cT[:, 2, :],
                                              start=True, stop=True)], reads=[wkvb, cTb], writes=[ab])
                    cx.op(cx.act, lambda e: e.copy(out=KT[h][0:64, cs], in_=a[0:64, :]), reads=[ab], writes=[KTb[h]])
                a, ab = gp[gi % 3], gpb[gi % 3]; gi += 1
                b2, b2b = gp[gi % 3], gpb[gi % 3]; gi += 1
                proj_T(cx, a[64:96, :], ab, w, wb, 512, 32, hc, hcb)
                proj_T(cx, b2[64:96, :], b2b, w, wb, 544, 32, hc, hcb)
                rope.apply([KT[0][64:96, cs], KT[1][64:96, cs]], [KTb[0], KTb[1]], a[64:96, :], ab, b2[64:96, :], b2b,
                           tmp, tmpb)
                for sub in range(4):
                    blk = ci * 4 + sub
                    cx.mm([lambda e: e.matmul(out=vp[:, 0:128], lhsT=cT[:, 2, sub * 128:(sub + 1) * 128],
                                              rhs=wkv[:, 0, 128:256], start=True, stop=True)],
                          reads=[cTb, wkvb], writes=[vpb])
                    cx.op(cx.dve, lambda e: e.tensor_copy(out=V[:, blk, :, 0:64],
                                                          in_=vp[:, 0:128].rearrange("p (h d) -> p h d", h=2)),
                          reads=[vpb], writes=[Vb])
        with cx.phase():
            s_ps = [[cx.psum("dS%d_%d" % (s, i), [128, 512], F32) for i in range(2)] for s in range(2)]
            s_psb = [[cx.pbuf("dS") for i in range(2)] for s in range(2)]
            pts = [[cx.sbuf("dP%d_%d" % (s, i), [128, 512], BF16) for i in range(2)] for s in range(2)]
            ptb = [[cx.buf("dP") for i in range(2)] for s in range(2)]
            streams = []
            for h in range(2):
                bank = cx.psum("dO%d" % h, [128, 4, 128], F32)
                bb = cx.pbuf("dO")
                streams.append(Stream(
                    qt=lambda qc, h=h: QT[h][:, qc * 512:(qc + 1) * 512], qtb=QTb[h],
                    kt=lambda kb, h=h: KT[h][:, kb * 128:(kb + 1) * 128], ktb=KTb[h],
                    v=lambda kb, h=h: V[:, kb, h, :], vb=Vb, ncol=65,
                    accs=[bank[:, j, 0:65] for j in range(4)], accb=[bb] * 4,
                    first_of_bank=[True, False, False, False]))
            rr = cx.sbuf("d_rr", [128, 2], F32); rrb = cx.buf("d_rr")
            oo = cx.sbuf("d_oo", [128, 128], F32); oob = cx.buf("d_oo")
            yy = [cx.sbuf("d_yy%d" % i, [128, 128], BF16) for i in range(2)]
            yyb = [cx.buf("d_yy") for i in range(2)]

            def epilogue(qc):
                for j in range(4):
                    blk = qc * 4 + j
                    for h in range(2):
                        acc, accb = streams[h].accs[j], streams[h].accb[j]
                        cx.op(cx.dve, lambda e: e.reciprocal(out=rr[:, h:h + 1], in_=acc[:, 64:65]), reads=[accb], writes=[rrb])
                        cx.op(cx.dve, lambda e: e.tensor_scalar(out=oo[:, h * 64:(h + 1) * 64], in0=acc[:, 0:64],
                                                                scalar1=rr[:, h:h + 1], scalar2=None, op0=ALU.mult),
                              reads=[accb, rrb], writes=[oob])
                    p = blk % 2
                    cx.op(cx.pool, lambda e: e.tensor_tensor(out=yy[p][:], in0=oo[:], in1=G[:, blk, :], op=ALU.mult),
                          reads=[oob, Gb], writes=[yyb[p]])
                    cx.dma(cx.sp, y_out[blk * 128:(blk + 1) * 128, ycol0:ycol0 + 128], yy[p][:], reads=[yyb[p]])

            causal_attention(cx, streams, D_SCALE, epilogue, s_ps, s_psb, pts, ptb, qchunks=DBG_QCH, diag_mask=(tri_kq, (ident, identb)))


def mixer_b(cx, hT, pos, wB, ropeB, dect, rtab, y_out, ycol0):
    with cx.phase():
        ident, identb = _consts(cx)
        w, wb = load_weight_bf16(cx, wB, D, 512, "wB")
        QT = cx.sbuf("bQT", [64, T], BF16); QTb = cx.buf("bQT")
        KT = cx.sbuf("bKT", [64, T], BF16); KTb = cx.buf("bKT")
        V = cx.sbuf("bV", [128, NBLK, 128], BF16); Vb = cx.buf("bV")
        G = cx.sbuf("bG", [128, NBLK, 128], BF16); Gb = cx.buf("bG")
        rc, rcb = _load_small(cx, ropeB, [128, 2], "ropeB")
        dec, decb = _load_small(cx, dect, [128, 128], "dect")
        rt, rtb = _load_small(cx, rtab, [128, 4], "rtab")
        with cx.phase():
            hs = HStream(cx, hT)
            rope = Rope(cx, pos, rc, rcb, 0, 64)
            gp = [cx.psum("bgp%d" % i, [128, 512], F32) for i in range(4)]
            gpb = [cx.pbuf("bgp") for i in range(4)]
            pk = [cx.psum("bpk%d" % i, [128, 512], F32) for i in range(2)]
            pkb = [cx.pbuf("bpk") for i in range(2)]
            sg = cx.sbuf("b_sg", [128, 128], F32); sgb = cx.buf("b_sg")
            tmp = [cx.sbuf("b_tmp%d" % i, [128, 512], F32) for i in range(2)]
            tmpb = [cx.buf("b_tmp") for i in range(2)]
            for ci in range(NCH):
                cs = slice(ci * 512, (ci + 1) * 512)
                hc, hcb = hs.load(ci)
                rope.compute(ci)
                for k in range(4):
                    proj_T(cx, gp[k][0:64, :], gpb[k], w, wb, 64 * k, 64, hc, hcb)
                rope.apply([QT[:, cs]], [QTb], gp[0][0:64, :], gpb[0], gp[1][0:64, :], gpb[1], tmp, tmpb)
                rope.apply([KT[:, cs]], [KTb], gp[2][0:64, :], gpb[2], gp[3][0:64, :], gpb[3], tmp, tmpb)
                for sub in range(4):
                    blk = ci * 4 + sub
                    p = sub % 2
                    proj_tok(cx, pk[p][:, 0:256], pkb[p], w, wb, 256, 256, hc, hcb, sub)
                    cx.op(cx.dve, lambda e: e.tensor_copy(out=V[:, blk, :], in_=pk[p][:, 0:128]),
                          reads=[pkb[p]], writes=[Vb])
                    silu_ps(cx, G[:, blk, :], Gb, pk[p][:, 128:256], pkb[p], sg, sgb)
        with cx.phase():
            sT = cx.psum("b_sT", [128, 512], F32); sTb = cx.pbuf("b_sT")
            kz_ps = cx.psum("b_kzp", [128, 8, 128], BF16); kz_psb = cx.pbuf("b_kzp")
            u_ps = cx.psum("b_u", [128, 512], F32); u_psb = cx.pbuf("b_u")
            o_ps = [cx.psum("b_o%d" % i, [128, 512], F32) for i in range(2)]
            o_psb = [cx.pbuf("b_o") for i in range(2)]
            sTs = cx.sbuf("b_sTs", [128, 128], BF16); sTsb = cx.buf("b_sTs")
            kz = cx.sbuf("b_kz", [128, 64], BF16); kzb = cx.buf("b_kz")
            st = cx.sbuf("b_state", [64, 128], F32); stb = cx.buf("b_state")
            stbf = cx.sbuf("b_statebf", [64, 128], BF16); stbfb = cx.buf("b_statebf")
            mv = cx.sbuf("b_mv", [128, 8], F32); mvb = cx.buf("b_mv")
            stats = cx.sbuf("b_stats", [128, 8], F32); statsb = cx.buf("b_stats")
            oo = cx.sbuf("b_oo", [128, 128], F32); oob = cx.buf("b_oo")
            yy = [cx.sbuf("b_yy%d" % i, [128, 128], BF16) for i in range(2)]
            yyb = [cx.buf("b_yy") for i in range(2)]
            cx.op(cx.dve, lambda e: e.memset(st[:], 0.0), writes=[stb])
            cx.op(cx.dve, lambda e: e.memset(stbf[:], 0.0), writes=[stbfb])
            for n in range(NBLK):
                ns = slice(n * 128, (n + 1) * 128)
                p = n % 2
                cx.mm([lambda e: e.matmul(out=sT[:, 0:128], lhsT=KT[:, ns], rhs=QT[:, ns], start=True, stop=True)],
                      reads=[KTb, QTb], writes=[sTb])
                cx.op(cx.dve, lambda e: e.tensor_tensor(out=sTs[:], in0=sT[:, 0:128], in1=dec[:], op=ALU.mult),
                      reads=[sTb, decb], writes=[sTsb])
                o, ob = o_ps[p], o_psb[p]
                cx.mm([lambda e: e.matmul(out=o[:, 0:128], lhsT=sTs[:], rhs=V[:, n, :], start=True, stop=False),
                       lambda e: e.matmul(out=o[:, 0:128], lhsT=QT[:, ns], rhs=stbf[:], start=False, stop=True)],
                      reads=[sTsb, Vb, QTb, stbfb], writes=[ob])
                cx.mm([lambda e: e.transpose(out=kz_ps[:, 0, 0:64], in_=KT[:, ns], identity=ident[0:64, 0:64])],
                      reads=[KTb, identb], writes=[kz_psb])
                cx.op(cx.dve, lambda e: e.tensor_scalar(out=kz[:], in0=kz_ps[:, 0, 0:64], scalar1=rt[:, 0:1], scalar2=None,
                                                        op0=ALU.mult), reads=[kz_psb, rtb], writes=[kzb])
                cx.mm([lambda e: e.matmul(out=u_ps[0:64, 0:128], lhsT=kz[:], rhs=V[:, n, :], start=True, stop=True)],
                      reads=[kzb, Vb], writes=[u_psb])
                cx.op(cx.dve, lambda e: e.scalar_tensor_tensor(out=st[:], in0=st[:], scalar=rt[0:64, 2:3], in1=u_ps[0:64, 0:128],
                                                               op0=ALU.mult, op1=ALU.add),
                      reads=[stb, rtb, u_psb], writes=[stb])
                cx.op(cx.pool, lambda e: e.tensor_copy(out=stbf[:], in_=st[:]), reads=[stb], writes=[stbfb])
                cx.op(cx.dve, lambda e: e.bn_stats(out=stats[:, 0:6], in_=o[:, 0:128]), reads=[ob], writes=[statsb])
                cx.op(cx.dve, lambda e: e.bn_aggr(out=mv[:, 0:2], in_=stats[:, 0:6]), reads=[statsb], writes=[mvb])
                cx.op(cx.dve, lambda e: e.tensor_tensor(out=mv[:, 1:2], in0=mv[:, 1:2], in1=rt[:, 1:2], op=ALU.add),
                      reads=[mvb, rtb], writes=[mvb])
                cx.op(cx.act, lambda e: e.activation(out=mv[:, 1:2], in_=mv[:, 1:2], func=AF.Sqrt), reads=[mvb], writes=[mvb])
                cx.op(cx.dve, lambda e: e.reciprocal(out=mv[:, 1:2], in_=mv[:, 1:2]), reads=[mvb], writes=[mvb])
                cx.op(cx.dve, lambda e: e.tensor_scalar(out=oo[:], in0=o[:, 0:128], scalar1=mv[:, 0:1], scalar2=mv[:, 1:2],
                                                        op0=ALU.subtract, op1=ALU.mult), reads=[ob, mvb], writes=[oob])
                cx.op(cx.pool, lambda e: e.tensor_tensor(out=yy[p][:], in0=oo[:], in1=G[:, n, :], op=ALU.mult),
                      reads=[oob, Gb], writes=[yyb[p]])
                cx.dma(cx.sp, y_out[n * 128:(n + 1) * 128, ycol0:ycol0 + 128], yy[p][:], reads=[yyb[p]])


TOPK = 256
NBIS = 17


def mixer_a(cx, hT, wA, y_out, ycol0):
    with cx.phase():
        ident, identb = _consts(cx)
        w, wb = load_weight_bf16(cx, wA, D, 900, "wA")
        QT = cx.sbuf("aQT", [128, T], BF16); QTb = cx.buf("aQT")
        KT = cx.sbuf("aKT", [128, T], BF16); KTb = cx.buf("aKT")
        V = cx.sbuf("aV", [128, NBLK, 2, 65], BF16); Vb = cx.buf("aV")
        G = cx.sbuf("aG", [128, NBLK, 128], BF16); Gb = cx.buf("aG")
        qiT = cx.sbuf("aqiT", [128, 2, T], BF16); qiTb = cx.buf("aqiT")
        kiT = cx.sbuf("akiT", [128, T], BF16); kiTb = cx.buf("akiT")
        Wi = cx.sbuf("aWi", [128, NBLK, 4], F32); Wib = cx.buf("aWi")
        cx.op(cx.pool, lambda e: e.memset(V[:, :, :, 64:65], 1.0), writes=[Vb])
        _, (tqk, tqkb), (nqk, nqkb) = _tri_masks(cx)
        with cx.phase():
            hs = HStream(cx, hT)
            gp = [cx.psum("agp%d" % i, [128, 512], F32) for i in range(3)]
            gpb = [cx.pbuf("agp") for i in range(3)]
            pk = [cx.psum("apk%d" % i, [128, 512], F32) for i in range(2)]
            pkb = [cx.pbuf("apk") for i in range(2)]
            sg = cx.sbuf("a_sg", [128, 128], F32); sgb = cx.buf("a_sg")
            gi = 0
            for ci in range(NCH):
                cs = slice(ci * 512, (ci + 1) * 512)
                hc, hcb = hs.load(ci)
                for col0, dst, dstb in ((0, QT[:, cs], QTb), (128, KT[:, cs], KTb), (516, qiT[:, 0, cs], qiTb),
                                        (644, qiT[:, 1, cs], qiTb), (772, kiT[:, cs], kiTb)):
                    a, ab = gp[gi % 3], gpb[gi % 3]; gi += 1
                    proj_T(cx, a[:], ab, w, wb, col0, 128, hc, hcb)
                    if gi % 2:
                        cx.op(cx.act, lambda e: e.copy(out=dst, in_=a[:]), reads=[ab], writes=[dstb])
                    else:
                        cx.op(cx.dve, lambda e: e.tensor_copy(out=dst, in_=a[:]), reads=[ab], writes=[dstb])
                for sub in range(4):
                    blk = ci * 4 + sub
                    p = sub % 2
                    proj_tok(cx, pk[p][:, 0:260], pkb[p], w, wb, 256, 260, hc, hcb, sub)
                    cx.op(cx.dve, lambda e: e.tensor_copy(out=V[:, blk, :, 0:64],
                                                          in_=pk[p][:, 0:128].rearrange("p (h d) -> p h d", h=2)),
                          reads=[pkb[p]], writes=[Vb])
                    cx.op(cx.dve, lambda e: e.tensor_copy(out=Wi[:, blk, :], in_=pk[p][:, 256:260]),
                          reads=[pkb[p]], writes=[Wib])
                    silu_ps(cx, G[:, blk, :], Gb, pk[p][:, 128:256], pkb[p], sg, sgb)
        with cx.phase():
            s_ps = [[cx.psum("aS%d_%d" % (s, i), [128, 512], F32) for i in range(2)] for s in range(2)]
            s_psb = [[cx.pbuf("aS") for i in range(2)] for s in range(2)]
            pts = [[cx.sbuf("aP%d_%d" % (s, i), [128, 128], BF16) for i in range(2)] for s in range(2)]
            ptb = [[cx.buf("aP") for i in range(2)] for s in range(2)]
            obank = cx.psum("aO", [128, 2, 256], F32); obb = cx.pbuf("aO")
            xps = [cx.psum("aX%d" % i, [128, 512], F32) for i in range(3)]
            xpb = [cx.pbuf("aX") for i in range(3)]
            I = cx.sbuf("aI", [128, T], F32); Ib = cx.buf("aI")
            MB = cx.sbuf("aMB", [128, T], BF16); MBb = cx.buf("aMB")
            junk = cx.sbuf("a_junk", [128, 2048], BF16); junkb = cx.buf("a_junk")
            xt = cx.sbuf("a_xt", [128, 512], F32); xtb = cx.buf("a_xt")
            bs = cx.sbuf("a_bs", [128, 8], F32); bsb = cx.buf("a_bs")
            streams = []
            for h in range(2):
                streams.append(Stream(
                    qt=lambda qc, h=h: QT[64 * h:64 * h + 64, qc * 128:(qc + 1) * 128], qtb=QTb,
                    kt=lambda kb, h=h: KT[64 * h:64 * h + 64, kb * 128:(kb + 1) * 128], ktb=KTb,
                    v=lambda kb, h=h: V[:, kb, h, :], vb=Vb, ncol=65,
                    accs=[obank[:, h, 0:65]], accb=[obb], first_of_bank=[h == 0]))
            rr = cx.sbuf("a_rr", [128, 2], F32); rrb = cx.buf("a_rr")
            oo = cx.sbuf("a_oo", [128, 128], F32); oob = cx.buf("a_oo")
            yy = [cx.sbuf("a_yy%d" % i, [128, 128], BF16) for i in range(2)]
            yyb = [cx.buf("a_yy") for i in range(2)]
            xi = [0]

            def prologue(qb):
                nk = (qb + 1) * 128
                qs = slice(qb * 128, (qb + 1) * 128)
                for k0 in range(0, nk, 512):
                    wd = min(512, nk - k0)
                    ks = slice(k0, k0 + wd)
                    for h in range(4):
                        x, xb = xps[xi[0] % 3], xpb[xi[0] % 3]; xi[0] += 1
                        rows = slice(64 * (h % 2), 64 * (h % 2) + 64)
                        cx.mm([lambda e: e.matmul(out=x[:, 0:wd], lhsT=qiT[rows, h // 2, qs], rhs=kiT[rows, ks],
                                                  start=True, stop=True)], reads=[qiTb, kiTb], writes=[xb])
                        if h == 0:
                            cx.op(cx.dve, lambda e: e.tensor_scalar(out=I[:, ks], in0=x[:, 0:wd], scalar1=0.0,
                                                                    scalar2=Wi[:, qb, h:h + 1], op0=ALU.max, op1=ALU.mult),
                                  reads=[xb, Wib], writes=[Ib])
                        else:
                            cx.op(cx.dve, lambda e: e.tensor_scalar(out=xt[:, 0:wd], in0=x[:, 0:wd], scalar1=0.0,
                                                                    scalar2=Wi[:, qb, h:h + 1], op0=ALU.max, op1=ALU.mult),
                                  reads=[xb, Wib], writes=[xtb])
                            cx.op(cx.pool, lambda e: e.tensor_tensor(out=I[:, ks], in0=I[:, ks], in1=xt[:, 0:wd], op=ALU.add),
                                  reads=[xtb, Ib], writes=[Ib])
                lo, hi, mid, cnt, ge, dd = (bs[:, i:i + 1] for i in range(6))
                if qb >= 2:
                    cx.op(cx.dve, lambda e: e.tensor_reduce(out=hi, in_=I[:, 0:nk], axis=AX.X, op=ALU.max,
                                                            apply_absolute_value=True), reads=[Ib], writes=[bsb])
                    cx.op(cx.dve, lambda e: e.tensor_scalar(out=lo, in0=hi, scalar1=-1.0, scalar2=None, op0=ALU.mult),
                          reads=[bsb], writes=[bsb])
                cx.op(cx.dve, lambda e: e.tensor_tensor(out=I[:, qs], in0=I[:, qs], in1=tqk[:], op=ALU.mult),
                      reads=[Ib, tqkb], writes=[Ib])
                cx.op(cx.dve, lambda e: e.tensor_tensor(out=I[:, qs], in0=I[:, qs], in1=nqk[:], op=ALU.add),
                      reads=[Ib, nqkb], writes=[Ib])
                if qb >= 2:
                    for it in range(NBIS):
                        cx.op(cx.dve, lambda e: e.tensor_tensor(out=mid, in0=lo, in1=hi, op=ALU.add), reads=[bsb], writes=[bsb])
                        cx.op(cx.dve, lambda e: e.tensor_scalar(out=mid, in0=mid, scalar1=0.5, scalar2=None, op0=ALU.mult),
                              reads=[bsb], writes=[bsb])
                        for a0 in range(0, nk, 2048):
                            wd = min(2048, nk - a0)
                            cx.op(cx.dve, lambda e: e.tensor_scalar(out=junk[:, 0:wd], in0=I[:, a0:a0 + wd], scalar1=mid,
                                                                    scalar2=(None if a0 == 0 else cnt), op0=ALU.is_ge,
                                                                    op1=ALU.add, accum_out=cnt),
                                  reads=[Ib, bsb], writes=[junkb, bsb])
                        cx.op(cx.dve, lambda e: e.tensor_scalar(out=ge, in0=cnt, scalar1=TOPK - 0.5, scalar2=None, op0=ALU.is_ge),
                              reads=[bsb], writes=[bsb])
                        cx.op(cx.dve, lambda e: e.tensor_tensor(out=dd, in0=mid, in1=lo, op=ALU.subtract), reads=[bsb], writes=[bsb])
                        cx.op(cx.dve, lambda e: e.scalar_tensor_tensor(out=lo, in0=dd, scalar=ge, in1=lo, op0=ALU.mult, op1=ALU.add),
                              reads=[bsb], writes=[bsb])
                        cx.op(cx.dve, lambda e: e.tensor_tensor(out=dd, in0=hi, in1=mid, op=ALU.subtract), reads=[bsb], writes=[bsb])
                        cx.op(cx.dve, lambda e: e.scalar_tensor_tensor(out=hi, in0=dd, scalar=ge, in1=mid, op0=ALU.mult, op1=ALU.add),
                              reads=[bsb], writes=[bsb])
                    cx.op(cx.dve, lambda e: e.tensor_scalar(out=MB[:, 0:nk], in0=I[:, 0:nk], scalar1=lo, scalar2=MASK_NEG,
                                                            op0=ALU.is_lt, op1=ALU.mult), reads=[Ib, bsb], writes=[MBb])
                else:
                    cx.op(cx.dve, lambda e: e.tensor_scalar(out=MB[:, 0:nk], in0=I[:, 0:nk], scalar1=-1e29, scalar2=MASK_NEG,
                                                            op0=ALU.is_lt, op1=ALU.mult), reads=[Ib], writes=[MBb])

            def extra_mask(qc, kb, s_tile):
                return ([lambda e: e.matmul(out=s_tile[:, 0:128], lhsT=MB[:, kb * 128:(kb + 1) * 128], rhs=ident[:],
                                            start=False, stop=True)], [MBb, identb])

            def epilogue(qb):
                for h in range(2):
                    acc = streams[h].accs[0]
                    cx.op(cx.dve, lambda e: e.reciprocal(out=rr[:, h:h + 1], in_=acc[:, 64:65]), reads=[obb], writes=[rrb])
                    cx.op(cx.dve, lambda e: e.tensor_scalar(out=oo[:, h * 64:(h + 1) * 64], in0=acc[:, 0:64],
                                                            scalar1=rr[:, h:h + 1], scalar2=None, op0=ALU.mult),
                          reads=[obb, rrb], writes=[oob])
                p = qb % 2
                cx.op(cx.pool, lambda e: e.tensor_tensor(out=yy[p][:], in0=oo[:], in1=G[:, qb, :], op=ALU.mult),
                      reads=[oob, Gb], writes=[yyb[p]])
                cx.dma(cx.sp, y_out[qb * 128:(qb + 1) * 128, ycol0:ycol0 + 128], yy[p][:], reads=[yyb[p]])

            causal_attention(cx, streams, C_SCALE, epilogue, s_ps, s_psb, pts, ptb, extra_mask=extra_mask,
                             qchunks=DBG_QCH, qw=1, diag_mask=None, prologue=prologue)


def build_l0():
    nc = bass.Bass("TRN2", target_bir_lowering=False)
    hT = nc.dram_tensor("hT", [D, T], BF16, kind="ExternalInput").ap()
    pos = nc.dram_tensor("pos", [T], I32, kind="ExternalInput").ap()
    wA = nc.dram_tensor("wA", [D, 900], F32, kind="ExternalInput").ap()
    wB = nc.dram_tensor("wB", [D, 512], F32, kind="ExternalInput").ap()
    ropeB = nc.dram_tensor("ropeB", [128, 2], F32, kind="ExternalInput").ap()
    dect = nc.dram_tensor("dect", [128, 128], F32, kind="ExternalInput").ap()
    rtab = nc.dram_tensor("rtab", [128, 4], F32, kind="ExternalInput").ap()
    y = nc.dram_tensor("y", [T, 256], BF16, kind="ExternalOutput").ap()
    with ExitStack() as es:
        cx = Ctx(nc, es)
        if "noA" not in DBG:
            mixer_a(cx, hT, wA, y, 0)
        if "noB" not in DBG:
            mixer_b(cx, hT, pos, wB, ropeB, dect, rtab, y, 128)
        cx.barrier()
    return nc


def build_l1():
    nc = bass.Bass("TRN2", target_bir_lowering=False)
    hT = nc.dram_tensor("hT", [D, T], BF16, kind="ExternalInput").ap()
    pos = nc.dram_tensor("pos", [T], I32, kind="ExternalInput").ap()
    wC = nc.dram_tensor("wC", [D, 512], F32, kind="ExternalInput").ap()
    wD = nc.dram_tensor("wD", [D, 576], F32, kind="ExternalInput").ap()
    wuq = nc.dram_tensor("wuq", [256, 384], F32, kind="ExternalInput").ap()
    wukv = nc.dram_tensor("wukv", [128, 256], F32, kind="ExternalInput").ap()
    lamp = nc.dram_tensor("lamp", [4, 64], F32, kind="ExternalInput").ap()
    subln = nc.dram_tensor("subln", [128], F32, kind="ExternalInput").ap()
    qn = nc.dram_tensor("qn", [256], F32, kind="ExternalInput").ap()
    kvn = nc.dram_tensor("kvn", [128], F32, kind="ExternalInput").ap()
    ropeD = nc.dram_tensor("ropeD", [128, 2], F32, kind="ExternalInput").ap()
    y = nc.dram_tensor("y", [T, 256], BF16, kind="ExternalOutput").ap()
    with ExitStack() as es:
        cx = Ctx(nc, es)
        if "noC" not in DBG:
            mixer_c(cx, hT, wC, lamp, subln, y, 0, LAM_INIT_L1)
        if "noD" not in DBG:
            mixer_d(cx, hT, pos, wD, wuq, wukv, qn, kvn, ropeD, y, 128)
        cx.barrier()
    return nc


def _cat(parts):
    return np.ascontiguousarray(np.concatenate(parts, axis=1))


def pack_even(w, g):
    sw = lambda m: np.concatenate([m[:, 32:], m[:, :32]], axis=1)
    a = lambda off: w[:, off + g * 128: off + (g + 1) * 128]
    wA = _cat([a(0), a(512), a(1024), a(1536), w[:, 2368:2372], w[:, 2048:2304], w[:, 2304:2368], w[:, 2304:2368]])
    bq = w[:, 2372 + g * 64: 2372 + (g + 1) * 64]
    bk = w[:, 2628 + g * 64: 2628 + (g + 1) * 64]
    wB = _cat([bq, sw(bq), bk, sw(bk), w[:, 2884 + g * 128: 2884 + (g + 1) * 128], w[:, 3396 + g * 128: 3396 + (g + 1) * 128]])
    return wA, wB


def pack_odd(w, w_uq, w_ukv, g):
    a = lambda off: w[:, off + g * 128: off + (g + 1) * 128]
    wC = _cat([a(0), a(512), a(1024), a(1536)])
    dkr = w[:, 2432:2464]
    wD = _cat([w[:, 2048:2304], w[:, 2304:2432], w[:, 2464 + g * 128: 2464 + (g + 1) * 128], dkr, dkr[:, 16:], dkr[:, :16]])
    qs, kn, vv = [], [], []
    for h in range(2):
        gh = 2 * g + h
        blk = w_uq[:, gh * 96:(gh + 1) * 96]
        qs += [blk, blk[:, :64], blk[:, 80:96], blk[:, 64:80]]
        kv = w_ukv[:, gh * 128:(gh + 1) * 128]
        kn.append(kv[:, :64])
        vv.append(kv[:, 64:])
    return wC, wD, _cat(qs), _cat(kn + vv)


def const_tables(g):
    ropeB = np.zeros((128, 2), np.float32)
    r = np.arange(64)
    ropeB[:64, 0] = (10000.0 ** (-(r % 32).astype(np.float32) / np.float32(32))).astype(np.float32)
    ropeB[:64, 1] = np.where(r < 32, -1.0, 1.0)
    ropeD = np.zeros((128, 2), np.float32)
    r = np.arange(32)
    ropeD[64:96, 0] = (10000.0 ** (-(r % 16).astype(np.float32) / np.float32(16))).astype(np.float32)
    ropeD[64:96, 1] = np.where(r < 16, -1.0, 1.0)
    gam = 1.0 - 2.0 ** (-5.0 - g)
    j = np.arange(128)
    dect = (0.125 * np.where(j[:, None] <= j[None, :], gam ** (-(j[:, None] + 1.0)), 0.0)).astype(np.float32)
    rtab = np.zeros((128, 4), np.float32)
    rtab[:, 0] = 0.125 * gam ** (127.0 - j)
    rtab[:, 1] = EPS / gam ** (2.0 * (j + 1.0))
    rtab[:, 2] = gam ** 128.0
    return ropeB, ropeD, dect, rtab


_PROGS = {}


def _prog(name, fn):
    if name not in _PROGS:
        _PROGS[name] = fn()
    return _PROGS[name]


def run_spmd(nc, in_maps):
    res = run_bass_kernel_spmd(nc, in_maps, core_ids=list(range(NCORES)))
    return res.results


def _gather_hT(res):
    return [np.ascontiguousarray(np.concatenate([res[b * 4 + s]["hT"] for s in range(4)], axis=1)) for b in range(B)]


def _gather_y(res):
    ys = []
    for b in range(B):
        yf = np.empty((T, D), dtype=res[0]["y"].dtype)
        for g in range(4):
            y = res[b * 4 + g]["y"]
            yf[:, g * 128:(g + 1) * 128] = y[:, 0:128]
            yf[:, 512 + g * 128:512 + (g + 1) * 128] = y[:, 128:256]
        ys.append(yf)
    return ys


def kernel(x, positions, pre_norm, post_norm, w_in_even, w_out_even, w_in_odd, diff_lambda, diff_subln,
           mla_q_norm, mla_kv_norm, mla_w_uq, mla_w_ukv, w_out_odd):
    f32 = lambda a: np.ascontiguousarray(np.asarray(a, dtype=np.float32))
    x = f32(x)
    positions = np.ascontiguousarray(np.asarray(positions, dtype=np.int32))
    pre_norm, post_norm = f32(pre_norm), f32(post_norm)
    w_in_even, w_out_even, w_in_odd, w_out_odd = f32(w_in_even)[0], f32(w_out_even)[0], f32(w_in_odd)[0], f32(w_out_odd)[0]
    lamp, subln = f32(diff_lambda)[0], f32(diff_subln)[0]
    qn, kvn, w_uq, w_ukv = f32(mla_q_norm)[0], f32(mla_kv_norm)[0], f32(mla_w_uq)[0], f32(mla_w_ukv)[0]
    tabs = [const_tables(g) for g in range(4)]
    xs = [np.ascontiguousarray(x[i // 4, (i % 4) * TS:(i % 4 + 1) * TS]) for i in range(NCORES)]

    res = run_spmd(_prog("p0", build_p0), [{"x": xs[i], "g": pre_norm[0]} for i in range(NCORES)])
    hT0 = _gather_hT(res)
    ins = []
    for i in range(NCORES):
        b, g = i // 4, i % 4
        wA, wB = pack_even(w_in_even, g)
        ropeB, ropeD, dect, rtab = tabs[g]
        ins.append({"hT": hT0[b], "pos": positions[b], "wA": wA, "wB": wB, "ropeB": ropeB, "dect": dect, "rtab": rtab})
    y0 = _gather_y(run_spmd(_prog("l0", build_l0), ins))
    ins = [{"x": xs[i], "y": np.ascontiguousarray(y0[i // 4][(i % 4) * TS:(i % 4 + 1) * TS]), "wo": w_out_even,
            "gp": post_norm[0], "gn": pre_norm[1]} for i in range(NCORES)]
    res = run_spmd(_prog("p3a", lambda: build_p3(False)), ins)
    x1 = [res[i]["xo"] for i in range(NCORES)]
    hT1 = _gather_hT(res)
    ins = []
    for i in range(NCORES):
        b, g = i // 4, i % 4
        wC, wD, wuq, wukv = pack_odd(w_in_odd, w_uq, w_ukv, g)
        ins.append({"hT": hT1[b], "pos": positions[b], "wC": wC, "wD": wD, "wuq": wuq, "wukv": wukv, "lamp": lamp,
                    "subln": subln, "qn": qn, "kvn": kvn, "ropeD": tabs[g][1]})
    y1 = _gather_y(run_spmd(_prog("l1", build_l1), ins))
    ins = [{"x": x1[i], "y": np.ascontiguousarray(y1[i // 4][(i % 4) * TS:(i % 4 + 1) * TS]), "wo": w_out_odd,
            "gp": post_norm[1]} for i in range(NCORES)]
    res = run_spmd(_prog("p3b", lambda: build_p3(True)), ins)
    out = np.empty((B, T, D), np.float32)
    for i in range(NCORES):
        out[i // 4, (i % 4) * TS:(i % 4 + 1) * TS] = res[i]["xo"]
    return out
```
